# Optimizing a Trainium2 kernel written in Bass

```python
import jax, jax.numpy as jnp
from jax import lax
import numpy as np

D_MODEL = 1024
BATCH = 8
SEQ = 4096
DEPTH = 1

POOL_WINDOWS = (2, 4, 8, 16)
POOL_GROUPS = len(POOL_WINDOWS)
POOL_WIDTH = D_MODEL // 4
POOL_GROUP_DIM = POOL_WIDTH // POOL_GROUPS
ATT_CONFIGS = ((128, 1), (512, 4), (2048, 16))
ATT_GROUPS = len(ATT_CONFIGS)
ATT_HEAD_DIM = 64
ATT_WIDTH = D_MODEL - POOL_WIDTH
ATT_HEADS = ATT_WIDTH // ATT_HEAD_DIM
ATT_HEADS_PER_GROUP = ATT_HEADS // ATT_GROUPS
ATT_OUT_WIDTH = ATT_HEADS_PER_GROUP * ATT_HEAD_DIM
ATT_BLOCK = 128
IN_PROJ_WIDTH = POOL_WIDTH + 3 * ATT_WIDTH
MIX_OUT_WIDTH = POOL_WIDTH + ATT_OUT_WIDTH
N_EXPERTS = 256
TOP_K = 8
N_EXPERT_GROUPS = 8
TOPK_GROUPS = 4
EXPERT_FF = D_MODEL // 4
SHARED_FF = D_MODEL // 4
ROUTED_SCALE = 2.5
MOE_BLOCK = 128
NORM_EPS = 1e-6

kernel_name = "hybrid_pool_dilatedattn_moe_adaln"


def rmsnorm(x, g):
    xf = x.astype(jnp.float32)
    y = xf * lax.rsqrt(jnp.mean(xf * xf, axis=-1, keepdims=True) + NORM_EPS)
    return (y * g.astype(jnp.float32)).astype(x.dtype)


def modulate(x, g, shift, scale):
    return rmsnorm(x, g) * (1 + scale[:, None, :]) + shift[:, None, :]


def pool_mixer(u, pool_w, pool_scale):
    B, S, _ = u.shape
    uf = u.astype(jnp.float32).reshape(B, S, POOL_GROUPS, POOL_GROUP_DIM)
    cs = jnp.cumsum(uf, axis=1)
    t = jnp.arange(S)
    outs = []
    for g, w in enumerate(POOL_WINDOWS):
        csg = cs[:, :, g]
        lag = jnp.pad(csg, ((0, 0), (w, 0), (0, 0)))[:, :S]
        cnt = jnp.minimum(t + 1, w).astype(jnp.float32)[None, :, None]
        outs.append((csg - lag) / cnt - uf[:, :, g])
    pooled = jnp.stack(outs, axis=2).astype(u.dtype)
    mixed = jnp.einsum('bsgc,gcd->bsgd', pooled, pool_w)
    return mixed.reshape(B, S, POOL_WIDTH) * pool_scale


def dilated_window_attention(q, k, v, window, dilation):
    B, S, H, E = q.shape
    steps = window // dilation
    span = dilation * ATT_BLOCK
    Sp = -(-S // span) * span
    L = Sp // dilation
    nb = L // ATT_BLOCK

    def to_blocks(a):
        a = jnp.pad(a, ((0, 0), (0, Sp - S), (0, 0), (0, 0)))
        a = a.reshape(B, L, dilation, H, E).transpose(0, 2, 1, 3, 4)
        return a.reshape(B, dilation, nb, ATT_BLOCK, H, E)

    def with_prev(a):
        prev = jnp.pad(a, ((0, 0), (0, 0), (1, 0), (0, 0), (0, 0), (0, 0)))[:, :, :nb]
        return jnp.concatenate([prev, a], axis=3)

    qb = to_blocks(q)
    kk = with_prev(to_blocks(k))
    vv = with_prev(to_blocks(v))
    s = jnp.einsum('brnqhe,brnkhe->brnhqk', qb, kk,
                   preferred_element_type=jnp.float32) * (E ** -0.5)
    i = jnp.arange(ATT_BLOCK)[:, None]
    j = jnp.arange(2 * ATT_BLOCK)[None, :]
    n = jnp.arange(nb)[:, None, None]
    dist = ATT_BLOCK + i - j
    valid = (dist >= 0) & (dist <= steps) & (n * ATT_BLOCK + j - ATT_BLOCK >= 0)
    s = jnp.where(valid[:, None], s, -jnp.inf)
    m = jnp.max(s, axis=-1, keepdims=True)
    p = jnp.exp(s - m)
    l = jnp.sum(p, axis=-1)
    o = jnp.einsum('brnhqk,brnkhe->brnqhe', p.astype(v.dtype), vv,
                   preferred_element_type=jnp.float32)
    o = o / jnp.swapaxes(l, 3, 4)[..., None]
    lse = jnp.swapaxes(m[..., 0] + jnp.log(l), 3, 4)

    def from_blocks(a):
        a = a.reshape((B, dilation, L) + a.shape[4:])
        a = jnp.swapaxes(a, 1, 2)
        return a.reshape((B, Sp) + a.shape[3:])[:, :S]

    return from_blocks(o), from_blocks(lse)


def attention_mixer(qkv):
    B, S, _ = qkv.shape
    qkv = qkv.reshape(B, S, 3, ATT_GROUPS, ATT_HEADS_PER_GROUP, ATT_HEAD_DIM)
    outs, lses = [], []
    for g, (w, d) in enumerate(ATT_CONFIGS):
        o, lse = dilated_window_attention(qkv[:, :, 0, g], qkv[:, :, 1, g], qkv[:, :, 2, g], w, d)
        outs.append(o)
        lses.append(lse)
    o = jnp.stack(outs, axis=0)
    alpha = jax.nn.softmax(jnp.stack(lses, axis=0), axis=0)
    out = jnp.sum(alpha[..., None] * o, axis=0)
    return out.reshape(B, S, ATT_OUT_WIDTH).astype(qkv.dtype)


def swiglu(h, w_gate, w_up, w_down):
    return (jax.nn.silu(h @ w_gate) * (h @ w_up)) @ w_down


def moe_ffn(h, w_router, router_bias, w_gate, w_up, w_down, ws_gate, ws_up, ws_down):
    B, S, D = h.shape
    N = B * S
    hf = h.reshape(N, D)
    scores = jax.nn.sigmoid((hf @ w_router).astype(jnp.float32))
    biased = scores + router_bias.astype(jnp.float32)
    grp = biased.reshape(N, N_EXPERT_GROUPS, N_EXPERTS // N_EXPERT_GROUPS)
    grp_score = jnp.sum(lax.top_k(grp, 2)[0], axis=-1)
    _, gidx = lax.top_k(grp_score, TOPK_GROUPS)
    gmask = jnp.sum(jax.nn.one_hot(gidx, N_EXPERT_GROUPS, dtype=jnp.float32), axis=1)
    emask = jnp.repeat(gmask, N_EXPERTS // N_EXPERT_GROUPS, axis=1) > 0
    _, eidx = lax.top_k(jnp.where(emask, biased, -jnp.inf), TOP_K)
    gates = jnp.take_along_axis(scores, eidx, axis=1)
    gates = gates / jnp.sum(gates, axis=-1, keepdims=True) * ROUTED_SCALE

    NK = N * TOP_K
    flat_e = eidx.reshape(NK)
    flat_tok = jnp.repeat(jnp.arange(N, dtype=jnp.int32), TOP_K)
    flat_w = gates.reshape(NK)
    order = jnp.argsort(flat_e, stable=True)
    se, stok, sw = flat_e[order], flat_tok[order], flat_w[order]
    counts = jnp.bincount(flat_e, length=N_EXPERTS)
    offs = jnp.cumsum(counts) - counts
    pcounts = (counts + MOE_BLOCK - 1) // MOE_BLOCK * MOE_BLOCK
    pend = jnp.cumsum(pcounts)
    poffs = pend - pcounts
    dest = poffs[se] + (jnp.arange(NK) - offs[se])
    P = NK + N_EXPERTS * MOE_BLOCK
    nblk = P // MOE_BLOCK
    row_tok = jnp.full((P,), N, jnp.int32).at[dest].set(stok)
    row_w = jnp.zeros((P,), jnp.float32).at[dest].set(sw)
    blk_e = jnp.minimum(jnp.searchsorted(pend, jnp.arange(nblk) * MOE_BLOCK, side='right'),
                        N_EXPERTS - 1).astype(jnp.int32)
    xpad = jnp.concatenate([hf, jnp.zeros((1, D), hf.dtype)], axis=0)

    def expert_block(args):
        e, toks, ws = args
        xb = xpad[toks]
        yb = swiglu(xb, w_gate[e], w_up[e], w_down[e])
        return yb * ws[:, None].astype(yb.dtype)

    ys = lax.map(expert_block, (blk_e, row_tok.reshape(nblk, MOE_BLOCK),
                                row_w.reshape(nblk, MOE_BLOCK)))
    routed = jax.ops.segment_sum(ys.reshape(P, D), row_tok, num_segments=N + 1)[:N]
    shared = swiglu(hf, ws_gate, ws_up, ws_down)
    return (routed + shared).reshape(B, S, D)


def setup_inputs(seed: int = 0) -> dict:
    key = jax.random.key(seed)
    ks = jax.random.split(key, 20)
    L, D, E, F = DEPTH, D_MODEL, N_EXPERTS, EXPERT_FF
    nrm = lambda k, shape, s: jax.random.normal(k, shape, jnp.float32) * s
    return {
        "x": nrm(ks[0], (BATCH, SEQ, D), 1.0),
        "c": nrm(ks[1], (BATCH, D), 1.0),
        "w_ada": nrm(ks[2], (L, D, 6 * D), 0.5 * D ** -0.5),
        "b_ada": nrm(ks[3], (L, 6 * D), 0.02),
        "g_mix": 1.0 + nrm(ks[4], (L, D), 0.05),
        "w_in": nrm(ks[5], (L, D, IN_PROJ_WIDTH), D ** -0.5),
        "pool_w": nrm(ks[6], (L, POOL_GROUPS, POOL_GROUP_DIM, POOL_GROUP_DIM), POOL_GROUP_DIM ** -0.5),
        "pool_scale": 1.0 + nrm(ks[7], (L, POOL_WIDTH), 0.1),
        "w_out": nrm(ks[8], (L, MIX_OUT_WIDTH, D), MIX_OUT_WIDTH ** -0.5),
        "g_ffn": 1.0 + nrm(ks[9], (L, D), 0.05),
        "w_router": nrm(ks[10], (L, D, E), D ** -0.5),
        "router_bias": nrm(ks[11], (L, E), 0.01),
        "w_gate": nrm(ks[12], (L, E, D, F), D ** -0.5),
        "w_up": nrm(ks[13], (L, E, D, F), D ** -0.5),
        "w_down": nrm(ks[14], (L, E, F, D), F ** -0.5),
        "ws_gate": nrm(ks[15], (L, D, SHARED_FF), D ** -0.5),
        "ws_up": nrm(ks[16], (L, D, SHARED_FF), D ** -0.5),
        "ws_down": nrm(ks[17], (L, SHARED_FF, D), SHARED_FF ** -0.5),
        "g_final": 1.0 + nrm(ks[18], (D,), 0.05),
    }


def reference(x, c, w_ada, b_ada, g_mix, w_in, pool_w, pool_scale, w_out, g_ffn,
              w_router, router_bias, w_gate, w_up, w_down, ws_gate, ws_up, ws_down, g_final):
    cs = jax.nn.silu(c.astype(jnp.float32))
    for l in range(DEPTH):
        mod = (cs @ w_ada[l].astype(jnp.float32) + b_ada[l].astype(jnp.float32)).astype(x.dtype)
        shift1, scale1, gate1, shift2, scale2, gate2 = jnp.split(mod, 6, axis=-1)
        h = modulate(x, g_mix[l], shift1, scale1)
        proj = h @ w_in[l]
        pool_out = pool_mixer(proj[..., :POOL_WIDTH], pool_w[l], pool_scale[l])
        attn_out = attention_mixer(proj[..., POOL_WIDTH:])
        mixed = jnp.concatenate([pool_out, attn_out], axis=-1) @ w_out[l]
        x = x + gate1[:, None, :] * mixed
        h = modulate(x, g_ffn[l], shift2, scale2)
        y = moe_ffn(h, w_router[l], router_bias[l], w_gate[l], w_up[l], w_down[l],
                    ws_gate[l], ws_up[l], ws_down[l])
        x = x + gate2[:, None, :] * y
    return rmsnorm(x, g_final)
```

```python
import numpy as np
import ml_dtypes
from contextlib import ExitStack
import concourse.bass as bass
import concourse.mybir as mybir
from concourse.bass_utils import run_bass_kernel_spmd

F32 = mybir.dt.float32
BF = mybir.dt.bfloat16
I32 = mybir.dt.int32
AF = mybir.ActivationFunctionType
ALU = mybir.AluOpType

NEG = -30000.0
BLK = 256
NBLK = 384
EBIG = 4096.0
NE = 256
S_TOK = 4096
D = 1024
NT = S_TOK // 128
EPS = 1e-6
ENGS = ("pe", "dve", "act", "pool", "sp")


class Sched:
    SAME_ENG_RAW_DIST = 6

    def __init__(self, nc):
        self.nc = nc
        self.streams = {e: [] for e in ENGS}
        self.writers = {}
        self.readers = {}
        self.dma_cnt = {}
        self.sems = {}

    def _add_deps(self, deps, toks, raw):
        for k, v in toks.items():
            cur = deps.get(k)
            if cur is None or v > cur[0]:
                deps[k] = (v, raw or (cur[1] if cur else False))
            elif raw and v == cur[0]:
                deps[k] = (v, True)

    def op(self, eng, fn, reads=(), writes=(), shared=(), dma=None):
        deps = {}
        for r in reads:
            self._add_deps(deps, self.writers.get(r, {}), True)
        for w in writes:
            self._add_deps(deps, self.writers.get(w, {}), False)
            self._add_deps(deps, self.readers.get(w, {}), False)
        for w in shared:
            self._add_deps(deps, self.readers.get(w, {}), False)
        idx = len(self.streams[eng])
        if dma is not None:
            cnt = self.dma_cnt.get(dma, 0) + 16
            self.dma_cnt[dma] = cnt
            tok = (("D", dma), cnt)
        else:
            tok = (("E", eng), idx)
        waits = []
        for k, (v, raw) in deps.items():
            if k[0] == "E" and k[1] == eng and dma is None:
                if eng == "pe":
                    continue
                if not raw or idx - v > self.SAME_ENG_RAW_DIST:
                    continue
            if k[0] == "E":
                self.streams[k[1]][v]["inc"] = True
            waits.append((k, v))
        self.streams[eng].append(dict(fn=fn, waits=waits, inc=False, dma=dma))
        for w in writes:
            self.writers[w] = {tok[0]: tok[1]}
            self.readers[w] = {}
        for w in shared:
            d = self.writers.setdefault(w, {})
            d[tok[0]] = max(d.get(tok[0], -1), tok[1])
        for r in reads:
            d = self.readers.setdefault(r, {})
            d[tok[0]] = max(d.get(tok[0], -1), tok[1])
        return tok

    def barrier_all(self):
        last = {}
        for e in ENGS:
            for i in range(len(self.streams[e]) - 1, -1, -1):
                o = self.streams[e][i]
                if o["fn"] is not None and o["dma"] is None:
                    last[e] = i
                    o["inc"] = True
                    break
        dmaw = [(("D", k), c) for k, c in self.dma_cnt.items()]
        for e in ENGS:
            waits = [(("E", f), i) for f, i in last.items() if f != e] + dmaw
            self.streams[e].append(dict(fn=None, waits=waits, inc=False, dma=None))
        self.writers = {}
        self.readers = {}

    def emit(self, stack):
        nc = self.nc
        for e in ENGS:
            self.sems[("E", e)] = stack.enter_context(nc.semaphore("s_" + e))
        for k in self.dma_cnt:
            self.sems[("D", k)] = stack.enter_context(nc.semaphore("d_" + str(k)))
        cum = {}
        for e in ENGS:
            c = 0
            arr = []
            for o in self.streams[e]:
                if o["inc"]:
                    c += 1
                arr.append(c)
            cum[e] = arr
        block = stack.enter_context(nc.Block())
        engobj = {"pe": "tensor", "dve": "vector", "act": "scalar", "pool": "gpsimd", "sp": "sync"}

        def body_for(e):
            def body(eng):
                waited = {}
                for o in self.streams[e]:
                    for k, v in o["waits"]:
                        val = cum[k[1]][v] if k[0] == "E" else v
                        if waited.get(k, 0) >= val:
                            continue
                        waited[k] = val
                        eng.wait_ge(self.sems[k], val)
                    if o["fn"] is None:
                        continue
                    ins = o["fn"](eng)
                    if o["dma"] is not None:
                        ins.then_inc(self.sems[("D", o["dma"])], 16)
                    elif o["inc"]:
                        ins.then_inc(self.sems[("E", e)], 1)
            return body

        for e in ENGS:
            getattr(block, engobj[e])(body_for(e))


def build_nc(dbg=(), stop_after=None, n_experts=NE):
    nc = bass.Bass("TRN2", target_bir_lowering=False)
    S = Sched(nc)
    main = ExitStack()

    def din(name, shape, dt=F32):
        return nc.dram_tensor(name, list(shape), dt, kind="ExternalInput").ap()

    x = din("x", [S_TOK, D])
    c_col = din("c_col", [128, 8])
    w_ada = din("w_ada", [D, 6 * D])
    b_col = din("b_col", [128, 16])
    b_row = din("b_row", [1, 6 * D])
    gmix_col = din("gmix_col", [128, 8])
    gffn_b = din("gffn_b", [128, D])
    gfin_b = din("gfin_b", [128, D])
    w_in = din("w_in", [D, 2560])
    pw_bd = din("pw_bd", [128, 2, 128])
    pscale_col = din("pscale_col", [128, 2])
    w_out = din("w_out", [512, D])
    w_router = din("w_router", [D, NE])
    rbias_b = din("rbias_b", [128, NE])
    w_gate = din("w_gate", [NE, D, 256])
    w_up = din("w_up", [NE, D, 256])
    w_down = din("w_down", [NE, 256, D])
    ws_gate = din("ws_gate", [D, 256])
    ws_up = din("ws_up", [D, 256])
    ws_down = din("ws_down", [256, D])
    ident_d = din("ident_bf", [128, 128], BF)
    tri_d = din("tri_bf", [128, 128], BF)
    onesb_d = din("ones_bf", [128, 128], BF)
    mask_d = din("mask_bf", [128, 2, 128], BF)
    onesf_d = din("ones_f", [128, 128])
    eC_d = din("eC_b", [128, NE])
    invw_d = din("invw_col", [128, 2])
    invcnt_d = din("invcnt", [128, 2, 16])
    jcol_d = din("jcol", [128, 3])

    out = nc.dram_tensor("out", [S_TOK, D], F32, kind="ExternalOutput").ap()
    xg = nc.dram_tensor("xg", [NBLK * BLK, D], BF, kind="Internal").ap()
    ysd = nc.dram_tensor("ysd", [NBLK * BLK, D], BF, kind="Internal").ap()
    h2d = nc.dram_tensor("h2d", [S_TOK, D], BF, kind="Internal").ap()
    dbg_out = {}

    def dbgt(name, shape, dt=F32):
        t = nc.dram_tensor("dbg_" + name, list(shape), dt, kind="ExternalOutput").ap()
        dbg_out[name] = t
        return t

    uniq = [0]

    def sb(stack, name, shape, dt):
        uniq[0] += 1
        return stack.enter_context(nc.sbuf_tensor(f"sb{uniq[0]}_{name}", list(shape), dt))

    def ps(stack, name, shape, dt=F32):
        uniq[0] += 1
        return stack.enter_context(nc.psum_tensor(f"ps{uniq[0]}_{name}", list(shape), dt))

    def E(eng, method, reads, writes, *args, shared=(), **kw):
        return S.op(eng, lambda e: getattr(e, method)(*args, **kw), reads=reads, writes=writes, shared=shared)

    def DMA(q, out_, in_, reads, writes, key, shared=()):
        return S.op(q, lambda e: e.dma_start(out=out_, in_=in_), reads=reads, writes=writes, shared=shared, dma=key)

    def MM(out_, lhsT, rhs, start, stop, reads, writes):
        return S.op("pe", lambda e: e.matmul(out_, lhsT=lhsT, rhs=rhs, start=start, stop=stop,
                                              skip_group_check=True), reads=reads, writes=writes)

    def TR(out_, in_, reads, writes):
        return S.op("pe", lambda e: e.transpose(out_, in_, ident[:]), reads=list(reads) + ["const"], writes=writes)

    with main:
        ident = sb(main, "ident", [128, 128], BF)
        tri = sb(main, "tri", [128, 128], BF)
        onesb = sb(main, "onesb", [128, 128], BF)
        maskb = sb(main, "maskb", [128, 2, 128], BF)
        onesf = sb(main, "onesf", [128, 128], F32)
        carryE = sb(main, "carryE", [128, NE], F32)
        rbias = sb(main, "rbias", [128, NE], F32)
        invw = sb(main, "invw", [128, 2], F32)
        invcnt = sb(main, "invcnt", [128, 2, 16], F32)
        pscale = sb(main, "pscale", [128, 2], F32)
        gs1c = sb(main, "gs1c", [128, 8], F32)
        modc = sb(main, "modc", [128, 16], F32)
        cs = sb(main, "cs", [128, 8], F32)
        pwbd = sb(main, "pwbd", [128, 2, 128], BF)
        attnT = sb(main, "attnT", [128, 2, S_TOK], BF)
        poolT = sb(main, "poolT", [128, 2, S_TOK], BF)
        slots_all = sb(main, "slots_all", [128, NT, 8], I32)
        gates_all = sb(main, "gates_all", [128, NT, 8], F32)
        stat = sb(main, "stat", [128, 4, NT], F32)
        jcol = sb(main, "jcol", [128, 3], F32)
        widx = sb(main, "widx", [128, NBLK], I32)

        for t_, d_ in ((ident, ident_d), (tri, tri_d), (onesb, onesb_d), (maskb, mask_d), (onesf, onesf_d),
                       (carryE, eC_d), (rbias, rbias_b), (invw, invw_d), (invcnt, invcnt_d),
                       (pscale, pscale_col), (jcol, jcol_d)):
            DMA("sp", t_[:], d_, [], ["const"], "ld_c", shared=["const"])
        DMA("pool", pwbd[:], pw_bd, [], [], "ld_c2", shared=["const"])

        def run_interleaved(gens, width):
            gens = iter(gens)
            active = []
            while True:
                while len(active) < width:
                    g_ = next(gens, None)
                    if g_ is None:
                        break
                    active.append(g_)
                if not active:
                    break
                for g_ in list(active):
                    try:
                        next(g_)
                    except StopIteration:
                        active.remove(g_)

        def adaln(blocks, tgt):
            with ExitStack() as st0:
                wa = [sb(st0, f"wa{i}", [128, 8, D], F32) for i in range(2)]
                pc = ps(st0, "pc", [128, 512])
                pr = ps(st0, "pr", [128, 1024])
                pb = ps(st0, "pb", [128, 1024])
                if 0 in blocks:
                    ccol = sb(st0, "ccol", [128, 8], F32)
                    bcol = sb(st0, "bcol", [128, 16], F32)
                    gmix = sb(st0, "gmix", [128, 8], F32)
                    DMA("sp", ccol[:], c_col, [], ["ccol"], "ld_c")
                    DMA("sp", bcol[:], b_col, [], ["bcol"], "ld_c")
                    DMA("sp", gmix[:], gmix_col, [], ["gmix"], "ld_c")
                    S.barrier_all()
                    E("act", "activation", ["ccol"], ["cs"], out=cs[:], in_=ccol[:], func=AF.Silu)
                else:
                    brow = sb(st0, "brow", [1, 6 * D], F32)
                    gffn = sb(st0, "gffn", [128, D], F32)
                    rowsb = sb(st0, "rowsb", [1, D], F32)
                    g1b = sb(st0, "g1b", [128, D], F32)
                    woutf = sb(st0, "woutf", [128, 4, D], F32)
                    wsdf = sb(st0, "wsdf", [128, 2, D], F32)
                    DMA("sp", brow[:], b_row, [], ["brow"], "ld_c")
                    DMA("sp", gffn[:], gffn_b, [], ["gffn"], "ld_c")
                    DMA("sp", woutf[:], w_out.rearrange("(k p) n -> p k n", p=128), [], ["woutf"], "ld_c")
                    DMA("sp", wsdf[:], ws_down.rearrange("(k p) n -> p k n", p=128), [], ["wsdf"], "ld_c")
                    S.barrier_all()
                    shift2_b, gs2_b, gate2_b, woutg, wsdg = tgt
                for j in blocks:
                    wj = wa[j % 2]
                    DMA("sp", wj[:], w_ada[:, j * D:(j + 1) * D].rearrange("(k p) n -> p k n", p=128),
                        [], [f"wa{j % 2}"], f"ld_wa{j % 2}")
                    if j < 2:
                        for jj in range(8):
                            for k in range(8):
                                MM(pc[:, jj:jj + 1], wj[:, k, jj * 128:(jj + 1) * 128], cs[:, k:k + 1], k == 0, k == 7,
                                   [f"wa{j % 2}", "cs"], ["pc"])
                        E("dve", "tensor_tensor", ["pc", "bcol"], [f"modc{j}"], out=modc[:, j * 8:(j + 1) * 8],
                          in0=pc[:, 0:8], in1=bcol[:, j * 8:(j + 1) * 8], op=ALU.add)
                    else:
                        for hf in range(2):
                            for k in range(8):
                                MM(pr[0:1, hf * 512:(hf + 1) * 512], cs[:, k:k + 1], wj[:, k, hf * 512:(hf + 1) * 512],
                                   k == 0, k == 7, [f"wa{j % 2}", "cs"], ["pr"])
                        E("dve", "tensor_tensor", ["pr", "brow"], ["rowsb"], out=rowsb[0:1, :], in0=pr[0:1, :],
                          in1=brow[0:1, j * D:(j + 1) * D], op=ALU.add)
                        for hf in range(2):
                            MM(pb[:, hf * 512:(hf + 1) * 512], onesf[0:1, :], rowsb[0:1, hf * 512:(hf + 1) * 512],
                               True, True, ["rowsb", "const"], ["pb"])
                        if j == 2:
                            E("act", "activation", ["pb"], ["g1b"], out=g1b[:], in_=pb[:], func=AF.Copy)
                            for c4 in range(4):
                                E("dve", "tensor_tensor", ["g1b", "woutf"], [], out=woutg[:, c4, :], in0=woutf[:, c4, :],
                                  in1=g1b[:], op=ALU.mult, shared=["woutg"])
                        elif j == 3:
                            E("act", "activation", ["pb"], ["shift2_b"], out=shift2_b[:], in_=pb[:], func=AF.Copy)
                        elif j == 4:
                            E("dve", "scalar_tensor_tensor", ["pb", "gffn"], ["gs2_b"], out=gs2_b[:], in0=pb[:],
                              scalar=1.0, in1=gffn[:], op0=ALU.add, op1=ALU.mult)
                        else:
                            E("act", "activation", ["pb"], ["gate2_b"], out=gate2_b[:], in_=pb[:], func=AF.Copy)
                            for c2 in range(2):
                                E("dve", "tensor_tensor", ["gate2_b", "wsdf"], [], out=wsdg[:, c2, :],
                                  in0=wsdf[:, c2, :], in1=gate2_b[:], op=ALU.mult, shared=["wsdg"])
                if 0 in blocks:
                    E("dve", "scalar_tensor_tensor", ["modc1", "gmix"], ["gs1c"], out=gs1c[:], in0=modc[:, 8:16],
                      scalar=1.0, in1=gmix[:], op0=ALU.add, op1=ALU.mult)
                    if "mod" in dbg:
                        d_ = dbgt("modc", [128, 24])
                        DMA("sp", d_[:, 0:16], modc[:], ["modc0", "modc1"], [], "dbg")
                        DMA("sp", d_[:, 16:24], gs1c[:], ["gs1c"], [], "dbg")
                elif "mod" in dbg:
                    d_ = dbgt("modr", [128, 3 * D])
                    DMA("sp", d_[:, 0:D], shift2_b[:], ["shift2_b"], [], "dbg")
                    DMA("sp", d_[:, D:2 * D], gs2_b[:], ["gs2_b"], [], "dbg")
                    DMA("sp", d_[:, 2 * D:3 * D], gate2_b[:], ["gate2_b"], [], "dbg")
                S.barrier_all()

        adaln([0, 1], None)
        if stop_after == 0:
            S.emit(main)
            return nc, dbg_out

        with ExitStack() as sa:
            KT = sb(sa, "KT", [128, 3, S_TOK], BF)
            Vt = sb(sa, "Vt", [128, 3, NT, 130], BF)
            hT = sb(sa, "hT", [128, 8, 2048], BF)
            QT = sb(sa, "QT", [128, 3, 2048], BF)
            wqkv = sb(sa, "wqkv", [128, 8, 9, 128], BF)
            wpool = sb(sa, "wpool", [128, 8, 256], BF)
            xt = [sb(sa, f"xt{i}", [128, D], F32) for i in range(2)]
            xs = [sb(sa, f"xs{i}", [128, D], BF) for i in range(2)]
            ubc = sb(sa, "ubc", [128, 2, 528], F32)
            lv = [sb(sa, f"lv{i}", [128, 2, 528], F32) for i in range(2)]
            fx = sb(sa, "fx", [128, 2, 16], F32)
            pooled = sb(sa, "pooled", [128, 2, 512], BF)
            Pt = [sb(sa, f"Pt{i}", [128, 2, 128], BF) for i in range(3)]
            rrow = [sb(sa, f"rrow{i}", [65, 512], F32) for i in range(2)]
            Rsb = [sb(sa, f"Rsb{i}", [64, 512], F32) for i in range(2)]
            stage = sb(sa, "stage", [64, 2048], BF)
            acc = ps(sa, "acc", [128, 2048])
            psS = [ps(sa, f"psS{i}", [128, 512]) for i in range(3)]
            psR = ps(sa, "psR", [128, 512])
            BANK = {0: ["S0"], 1: ["S1"], 2: ["S2"], 3: ["psR"]}
            PJ = {0: psS[0], 1: psS[1], 2: psS[2], 3: psR}

            E("pool", "memset", [], [], Vt[:, :, :, 64:65], 1.0, shared=["Vt"])
            E("pool", "memset", [], [], Vt[:, :, :, 129:130], 1.0, shared=["Vt"])
            E("pool", "memset", [], ["ubc"], ubc[:, :, 0:16], 0.0)
            mhalfA = sb(sa, "mhalfA", [128, 1], F32)
            E("pool", "memset", [], ["mhalfA"], mhalfA[:], -0.5)
            DMA("pool", wpool[:], w_in[:, 0:256].rearrange("(k p) n -> p k n", p=128), [], ["wpool"], "ld_wpool")

            for hp in range(2 if "skipA" not in dbg else 0):
                for ci in range(9):
                    which, g = divmod(ci, 3)
                    c0 = 256 + which * 768 + g * 256 + hp * 128
                    DMA("pool", wqkv[:, :, ci, :], w_in[:, c0:c0 + 128].rearrange("(k p) n -> p k n", p=128),
                        [], [f"wqkv{ci}"], f"ld_wg{ci % 4}" if ci < 4 else (f"ld_wu{ci % 4}" if ci < 8 else "ld_wd0"))
                for sbi in range(2):
                    t0 = sbi * 16
                    def bodyA1(tl):
                        ti = t0 + tl
                        b2 = ti % 2
                        xb_ = xt[b2]
                        xs_ = xs[b2]
                        pt_ = psS[b2][:].bitcast(BF)
                        R = lambda n: f"{n}{b2}"
                        DMA("sp", xb_[:], x[ti * 128:(ti + 1) * 128, :], [], [R("xt")], f"ld_x{b2}")
                        yield
                        E("act", "activation", [R("xt")], [R("xs"), R("ss")], out=xs_[:], in_=xb_[:],
                          func=AF.Square, accum_out=stat[:, 0, ti:ti + 1])
                        yield
                        E("dve", "tensor_scalar", [R("ss")], [R("ms")], out=stat[:, 1, ti:ti + 1],
                          in0=stat[:, 0, ti:ti + 1], scalar1=1.0 / D, scalar2=EPS, op0=ALU.mult, op1=ALU.add)
                        yield
                        E("pool", "tensor_tensor", [R("ms"), "mhalfA"], [R("rstd")], out=stat[:, 3, ti:ti + 1],
                          in0=stat[:, 1, ti:ti + 1], in1=mhalfA[:], op=ALU.pow)
                        yield
                        E("act", "activation", [R("xt"), R("rstd")], [R("xs")], out=xs_[:], in_=xb_[:],
                          func=AF.Copy, scale=stat[:, 3, ti:ti + 1])
                        yield
                        for k in range(8):
                            TR(pt_[:, k * 128:(k + 1) * 128], xs_[:, k * 128:(k + 1) * 128], [R("xs")], BANK[b2])
                        yield
                        for k in range(8):
                            E("dve", "tensor_scalar", BANK[b2] + ["gs1c", "modc0"], [],
                              out=hT[:, k, tl * 128:(tl + 1) * 128], in0=pt_[:, k * 128:(k + 1) * 128],
                              scalar1=gs1c[:, k:k + 1], scalar2=modc[:, k:k + 1], op0=ALU.mult, op1=ALU.add,
                              shared=["hT"])
                    run_interleaved((bodyA1(tl) for tl in range(16)), 2)
                    cnt = [0]

                    def nextpj():
                        i = cnt[0] % 4
                        cnt[0] += 1
                        return PJ[i], BANK[i]

                    for ci in range(6):
                        which, g = divmod(ci, 3)
                        for tc in range(4):
                            pj, pjn = nextpj()
                            for k in range(8):
                                MM(pj[:, :], wqkv[:, k, ci, :], hT[:, k, tc * 512:(tc + 1) * 512], k == 0, k == 7,
                                   [f"wqkv{ci}", "hT"], pjn)
                            if which == 0:
                                E("act", "activation", pjn, [], out=QT[:, g, tc * 512:(tc + 1) * 512], in_=pj[:, :],
                                  func=AF.Copy, shared=["QT"])
                            else:
                                E("act", "activation", pjn, [],
                                  out=KT[:, g, sbi * 2048 + tc * 512: sbi * 2048 + (tc + 1) * 512], in_=pj[:, :],
                                  func=AF.Copy, shared=["KT"])
                    for g in range(3):
                        for bi in range(16):
                            if g == 0:
                                vi = t0 + bi
                                lo, step = bi * 128, 1
                            elif g == 1:
                                r, nl = divmod(bi, 4)
                                vi = r * 8 + sbi * 4 + nl
                                lo, step = nl * 512 + r, 4
                            else:
                                r = bi
                                vi = r * 2 + sbi
                                lo, step = r, 16
                            pj, pjn = nextpj()
                            for k in range(8):
                                MM(pj[:, 0:128], hT[:, k, lo:lo + 127 * step + 1:step], wqkv[:, k, 6 + g, :], k == 0, k == 7,
                                   [f"wqkv{6 + g}", "hT"], pjn)
                            E("act", "activation", pjn, [],
                              out=Vt[:, g, vi, :].rearrange("p (h e) -> p h e", h=2)[:, :, 0:64],
                              in_=pj[:, 0:128].rearrange("p (h e) -> p h e", h=2), func=AF.Copy, shared=["Vt"])
                    if hp == 0:
                        for tc in range(4):
                            if not (sbi == 0 and tc == 0):
                                E("dve", "tensor_copy", ["ubc"], ["ubc"], out=ubc[:, :, 0:16], in_=ubc[:, :, 512:528])
                            for ch in range(2):
                                pj, pjn = nextpj()
                                for k in range(8):
                                    MM(pj[:, :], wpool[:, k, ch * 128:(ch + 1) * 128], hT[:, k, tc * 512:(tc + 1) * 512],
                                       k == 0, k == 7, ["wpool", "hT"], pjn)
                                E("act", "activation", pjn + ["ubc"], [], out=ubc[:, ch, 16:528], in_=pj[:, :],
                                  func=AF.Copy, shared=["ubc2"])
                            offs = (1, 3, 7, 15)
                            for g in range(4):
                                o_ = offs[g]
                                dst = lv[g % 2]
                                src = ubc if g == 0 else lv[(g - 1) % 2]
                                sh = 1 << g
                                E("dve", "tensor_tensor", ["ubc", "ubc2", f"lv{(g - 1) % 2}"], [f"lv{g % 2}"],
                                  out=dst[:, :, o_:528], in0=src[:, :, o_:528], in1=src[:, :, o_ - sh:528 - sh], op=ALU.add)
                                ch, gl = divmod(g, 2)
                                pr_ = slice(gl * 64, (gl + 1) * 64)
                                E("dve", "scalar_tensor_tensor", [f"lv{g % 2}", "ubc2", "const"], [], out=pooled[pr_, ch, :],
                                  in0=dst[pr_, ch, 16:528], scalar=invw[pr_, ch:ch + 1], in1=ubc[pr_, ch, 16:528],
                                  op0=ALU.mult, op1=ALU.subtract, shared=["pooled"])
                                if sbi == 0 and tc == 0:
                                    E("dve", "tensor_tensor", [f"lv{g % 2}", "const"], ["fx"], out=fx[pr_, ch, :],
                                      in0=dst[pr_, ch, 16:32], in1=invcnt[pr_, ch, :], op=ALU.mult)
                                    E("dve", "tensor_tensor", ["fx", "ubc2"], [], out=pooled[pr_, ch, 0:16],
                                      in0=fx[pr_, ch, :], in1=ubc[pr_, ch, 16:32], op=ALU.subtract,
                                      shared=["pooled"])
                            for ch in range(2):
                                pj, pjn = nextpj()
                                MM(pj[:, :], pwbd[:, ch, :], pooled[:, ch, :], True, True, ["pooled", "const"], pjn)
                                E("act", "activation", pjn + ["const"], [],
                                  out=poolT[:, ch, sbi * 2048 + tc * 512: sbi * 2048 + (tc + 1) * 512], in_=pj[:, :],
                                  func=AF.Copy, scale=pscale[:, ch:ch + 1], shared=["poolT"])
                    for hl in range(2):
                        hs = slice(hl * 64, (hl + 1) * 64)
                        first = [True] * 4
                        bctr = [0]

                        def bodyA3(g, bi):
                            if g == 0:
                                n = t0 + bi
                                qsl = slice(bi * 128, (bi + 1) * 128)
                                ksl = [slice((n - 1) * 128, n * 128), slice(n * 128, (n + 1) * 128)]
                                vis = [n - 1, n]
                            elif g == 1:
                                r, nl = divmod(bi, 4)
                                n = sbi * 4 + nl
                                qsl = slice(nl * 512 + r, nl * 512 + r + 509, 4)
                                ksl = [slice((n - 1) * 512 + r, (n - 1) * 512 + r + 509, 4),
                                       slice(n * 512 + r, n * 512 + r + 509, 4)]
                                vis = [r * 8 + n - 1, r * 8 + n]
                            else:
                                r = bi
                                n = sbi
                                qsl = slice(r, r + 2033, 16)
                                ksl = [slice((n - 1) * 2048 + r, (n - 1) * 2048 + r + 2033, 16),
                                       slice(n * 2048 + r, n * 2048 + r + 2033, 16)]
                                vis = [r * 2 + n - 1, r * 2 + n]
                            has_prev = n >= 1
                            bsel = bctr[0] % 3
                            bctr[0] += 1
                            pS = psS[bsel][:, 0:256].rearrange("p (a b) -> p a b", a=2)
                            pSn = f"S{bsel}"
                            Pb = Pt[bsel]
                            kbs = [0, 1] if has_prev else [1]
                            for kb in kbs:
                                MM(pS[:, kb, :], KT[hs, g, ksl[kb]], QT[hs, g, qsl], True, True, ["KT", "QT"], [pSn])
                            yield
                            k0 = kbs[0]
                            E("act", "activation", [pSn], [f"P{bsel}"], out=Pb[:, k0:2, :], in_=pS[:, k0:2, :],
                              func=AF.Exp, scale=0.125)
                            E("dve", "tensor_tensor", [f"P{bsel}", "const"], [f"P{bsel}"], out=Pb[:, k0:2, :],
                              in0=Pb[:, k0:2, :], in1=maskb[:, k0:2, :], op=ALU.mult)
                            yield
                            for kb in kbs:
                                lhs = Vt[:, g, vis[kb], hl * 65:hl * 65 + 65]
                                if g == 0:
                                    c_, o_ = divmod(bi, 4)
                                    cols = slice(c_ * 512 + o_ * 128, c_ * 512 + (o_ + 1) * 128)
                                    MM(acc[0:65, cols], lhs, Pb[:, kb, :], first[c_], False, ["Vt", f"P{bsel}"], ["acc"])
                                    first[c_] = False
                                elif g == 1:
                                    cols = slice(nl * 512 + r, nl * 512 + r + 509, 4)
                                    MM(acc[0:65, cols], lhs, Pb[:, kb, :], False, False, ["Vt", f"P{bsel}"], ["acc"])
                                else:
                                    for c_ in range(4):
                                        cols = slice(c_ * 512 + r, c_ * 512 + r + 497, 16)
                                        MM(acc[0:65, cols], lhs, Pb[:, kb, c_ * 32:(c_ + 1) * 32], False, False,
                                           ["Vt", f"P{bsel}"], ["acc"])

                        run_interleaved((bodyA3(g, bi) for g in range(3) for bi in range(16)), 3)
                        for c_ in range(4):
                            cs_ = slice(c_ * 512, (c_ + 1) * 512)
                            rr = rrow[c_ % 2]
                            E("act", "activation", ["acc"], [f"rrow{c_ % 2}"], out=rr[64:65, :], in_=acc[64:65, cs_], func=AF.Ln)
                            E("act", "activation", [f"rrow{c_ % 2}"], [f"rrow{c_ % 2}"], out=rr[64:65, :], in_=rr[64:65, :],
                              func=AF.Exp, scale=-1.0)
                            MM(psR[0:64, :], onesf[64:65, 0:64], rr[64:65, :], True, True, [f"rrow{c_ % 2}", "const"],
                               ["psR"])
                            E("act", "activation", ["psR"], [f"Rsb{c_ % 2}"], out=Rsb[c_ % 2][:, :], in_=psR[0:64, :],
                              func=AF.Copy)
                            E("dve", "tensor_tensor", ["acc", f"Rsb{c_ % 2}"], [], out=stage[:, cs_], in0=acc[0:64, cs_],
                              in1=Rsb[c_ % 2][:, :], op=ALU.mult, shared=["stage"])
                        DMA("sp", attnT[hs, hp, sbi * 2048:(sbi + 1) * 2048], stage[:, :], ["stage"], [], "st_attn",
                            shared=["attnT"])
            if "attn" in dbg:
                d_ = dbgt("attnT", [128, 2 * S_TOK], BF)
                DMA("sp", d_, attnT[:].rearrange("p a b -> p (a b)"), ["attnT"], [], "dbg")
                d_ = dbgt("poolT", [128, 2 * S_TOK], BF)
                DMA("sp", d_, poolT[:].rearrange("p a b -> p (a b)"), ["poolT"], [], "dbg")
                d_ = dbgt("KT", [128, 3 * S_TOK], BF)
                DMA("sp", d_, KT[:].rearrange("p a b -> p (a b)"), ["KT"], [], "dbg")
                d_ = dbgt("Vt", [128, 3 * NT * 130], BF)
                DMA("sp", d_, Vt[:].rearrange("p a b c -> p (a b c)"), ["Vt"], [], "dbg")
                d_ = dbgt("hT", [128, 8 * 2048], BF)
                DMA("sp", d_, hT[:].rearrange("p a b -> p (a b)"), ["hT"], [], "dbg")
            S.barrier_all()
        if stop_after == 1:
            S.emit(main)
            return nc, dbg_out
        shift2_b = sb(main, "shift2_b", [128, D], F32)
        gs2_b = sb(main, "gs2_b", [128, D], F32)
        gate2_b = sb(main, "gate2_b", [128, D], F32)
        woutg = sb(main, "woutg", [128, 4, D], BF)
        wsdg = sb(main, "wsdg", [128, 2, D], BF)
        selb_all = sb(main, "selb_all", [128, NT, NE], BF)
        adaln([2, 3, 4, 5], (shift2_b, gs2_b, gate2_b, woutg, wsdg))
        if stop_after == 1.5:
            S.emit(main)
            return nc, dbg_out

        with ExitStack() as sB:
            wr = sb(sB, "wr", [128, 8, NE], BF)
            wsg = sb(sB, "wsg", [128, 8, 256], BF)
            wsu = sb(sB, "wsu", [128, 8, 256], BF)
            mhalf = sb(sB, "mhalf", [128, 1], F32)
            NS = 3
            xt = [sb(sB, f"xtB{i}", [128, D], F32) for i in range(NS)]
            x1 = [sb(sB, f"x1_{i}", [128, D], F32) for i in range(NS)]
            t1 = [sb(sB, f"t1B{i}", [128, D], F32) for i in range(NS)]
            junk = [sb(sB, f"junkB{i}", [128, D], BF) for i in range(NS)]
            h2 = [sb(sB, f"h2_{i}", [128, D], BF) for i in range(NS)]
            h2T = [sb(sB, f"h2T{i}", [128, 8, 128], BF) for i in range(NS)]
            sc = [sb(sB, f"sc{i}", [128, NE], F32) for i in range(NS)]
            bia = [sb(sB, f"bia{i}", [128, NE], F32) for i in range(NS)]
            mb = [sb(sB, f"mb{i}", [128, NE], F32) for i in range(NS)]
            g0t = [sb(sB, f"g0t{i}", [128, NE], F32) for i in range(NS)]
            S1 = [sb(sB, f"S1{i}", [128, NE], F32) for i in range(NS)]
            junk2 = [sb(sB, f"junk2{i}", [128, NE], F32) for i in range(NS)]
            g8 = [sb(sB, f"g8{i}", [128, 8, 8], F32) for i in range(NS)]
            sm = [sb(sB, f"sm{i}", [128, 64], F32) for i in range(NS)]
            sg = [sb(sB, f"sgB{i}", [128, 2, 128], F32) for i in range(NS)]
            hsb = [sb(sB, f"hsb{i}", [128, 2, 128], BF) for i in range(NS)]
            bt = [sb(sB, f"btB{i}", [128, D], F32) for i in range(NS)]
            pm = ps(sB, "pm", [128, 1024])
            pd = ps(sB, "pd", [128, 1024])
            ptr = ps(sB, "ptrB", [128, 1024], BF)
            prt = ps(sB, "prt", [128, 1024])
            phg = ps(sB, "phg", [128, 512])

            DMA("pool", wr[:], w_router.rearrange("(k p) n -> p k n", p=128), [], ["wr"], "ld_wr")
            DMA("pool", wsg[:], ws_gate.rearrange("(k p) n -> p k n", p=128), [], ["wsg"], "ld_wsg")
            DMA("pool", wsu[:], ws_up.rearrange("(k p) n -> p k n", p=128), [], ["wsu"], "ld_wsu")
            E("pool", "memset", [], ["mhalf"], mhalf[:], -0.5)

            def bodyB(ti):
                q = ti % NS
                tsl = slice(ti * 128, (ti + 1) * 128)
                selb = selb_all[:, ti, :]
                xb_, x1_, t1_, jk_, h2_, h2T_, bt_ = xt[q], x1[q], t1[q], junk[q], h2[q], h2T[q], bt[q]
                sc_, bia_, mb_, g0_, S1_, j2_, g8_, sm_, sg_, hs_ = sc[q], bia[q], mb[q], g0t[q], S1[q], junk2[q], g8[q], sm[q], sg[q], hsb[q]
                R = lambda n: f"{n}{q}"
                DMA("sp", xb_[:], x[tsl, :], [], [R("xtB")], f"ld_x{q}")
                yield
                for hf in range(2):
                    for c4 in range(4):
                        lhs = poolT[:, c4, tsl] if c4 < 2 else attnT[:, c4 - 2, tsl]
                        MM(pm[:, hf * 512:(hf + 1) * 512], lhs, woutg[:, c4, hf * 512:(hf + 1) * 512], c4 == 0, c4 == 3,
                           ["poolT", "attnT", "woutg"], ["pm"])
                E("dve", "tensor_tensor", ["pm", R("xtB")], [R("x1_")], out=x1_[:], in0=pm[:], in1=xb_[:], op=ALU.add)
                yield
                E("act", "activation", [R("x1_")], [R("junkB"), R("ss")], out=jk_[:], in_=x1_[:], func=AF.Square,
                  accum_out=stat[:, 0, ti:ti + 1])
                yield
                E("dve", "tensor_scalar", [R("ss")], [R("ms")], out=stat[:, 1, ti:ti + 1], in0=stat[:, 0, ti:ti + 1],
                  scalar1=1.0 / D, scalar2=EPS, op0=ALU.mult, op1=ALU.add)
                yield
                E("pool", "tensor_tensor", [R("ms"), "mhalf"], [R("rstd")], out=stat[:, 3, ti:ti + 1],
                  in0=stat[:, 1, ti:ti + 1], in1=mhalf[:], op=ALU.pow)
                yield
                E("dve", "scalar_tensor_tensor", [R("x1_"), R("rstd"), "gs2_b"], [R("t1B")], out=t1_[:], in0=x1_[:],
                  scalar=stat[:, 3, ti:ti + 1], in1=gs2_b[:], op0=ALU.mult, op1=ALU.mult)
                yield
                E("pool", "tensor_tensor", [R("t1B"), "shift2_b"], [R("h2_")], out=h2_[:], in0=t1_[:], in1=shift2_b[:],
                  op=ALU.add)
                DMA("sp", h2d[tsl, :], h2_[:], [R("h2_")], [], f"st_h2{q}", shared=["h2d"])
                yield
                for k in range(8):
                    TR(ptr[:, k * 128:(k + 1) * 128], h2_[:, k * 128:(k + 1) * 128], [R("h2_")], ["ptrB"])
                E("act", "activation", ["ptrB"], [R("h2T")], out=h2T_[:].rearrange("p a b -> p (a b)"), in_=ptr[:],
                  func=AF.Copy)
                yield
                lg = prt[:, 0:256]
                cum = prt[:, 512:768]
                csum = prt[:, 768:1024]
                for k in range(8):
                    MM(lg, h2T_[:, k, :], wr[:, k, :], k == 0, k == 7, [R("h2T"), "wr"], ["lg"])
                E("act", "activation", ["lg"], [R("sc")], out=sc_[:], in_=lg, func=AF.Sigmoid)
                yield
                hg = phg[:, 0:256].rearrange("p (a b) -> p a b", a=2)
                hu = phg[:, 256:512].rearrange("p (a b) -> p a b", a=2)
                for fc in range(2):
                    for k in range(8):
                        MM(hg[:, fc, :], wsg[:, k, fc * 128:(fc + 1) * 128], h2T_[:, k, :], k == 0, k == 7,
                           [R("h2T"), "wsg"], ["phg"])
                for fc in range(2):
                    for k in range(8):
                        MM(hu[:, fc, :], wsu[:, k, fc * 128:(fc + 1) * 128], h2T_[:, k, :], k == 0, k == 7,
                           [R("h2T"), "wsu"], ["phg"])
                E("act", "activation", ["phg"], [R("sgB")], out=sg_[:], in_=hg, func=AF.Silu)
                E("dve", "tensor_tensor", [R("sgB"), "phg"], [R("hsb")], out=hs_[:], in0=sg_[:], in1=hu, op=ALU.mult)
                yield
                for hf in range(2):
                    for fc in range(2):
                        MM(pd[:, hf * 512:(hf + 1) * 512], hs_[:, fc, :], wsdg[:, fc, hf * 512:(hf + 1) * 512], fc == 0,
                           fc == 1, [R("hsb"), "wsdg"], ["pd"])
                E("dve", "tensor_tensor", ["pd", R("x1_")], [R("btB")], out=bt_[:], in0=pd[:], in1=x1_[:], op=ALU.add)
                DMA("sp", out[tsl, :], bt_[:], [R("btB")], [], f"st_b{q}", shared=["outd"])
                yield
                E("dve", "tensor_tensor", [R("sc"), "const"], [R("bia")], out=bia_[:], in0=sc_[:], in1=rbias[:], op=ALU.add)
                for g in range(8):
                    E("dve", "max", [R("bia")], [], out=g8_[:, g, :], in_=bia_[:, g * 32:(g + 1) * 32], shared=[R("g8")])
                yield
                E("dve", "tensor_tensor", [R("g8")], [R("grp")], out=sm_[:, 0:8], in0=g8_[:, :, 0], in1=g8_[:, :, 1],
                  op=ALU.add)
                yield
                E("dve", "max", [R("grp")], [R("m8")], out=sm_[:, 8:16], in_=sm_[:, 0:8])
                yield
                E("dve", "tensor_scalar", [R("grp"), R("m8")], [R("gmask")], out=sm_[:, 16:24], in0=sm_[:, 0:8],
                  scalar1=sm_[:, 11:12], scalar2=None, op0=ALU.is_ge)
                yield
                E("dve", "tensor_scalar", [R("gmask")], [R("pen")], out=sm_[:, 24:32], in0=sm_[:, 16:24], scalar1=1e30,
                  scalar2=-1e30, op0=ALU.mult, op1=ALU.add)
                bia3 = bia_[:].rearrange("p (g e) -> p g e", g=8)
                mb3 = mb_[:].rearrange("p (g e) -> p g e", g=8)
                E("dve", "tensor_tensor", [R("bia"), R("gmask")], [R("mb0")], out=mb3, in0=bia3,
                  in1=sm_[:, 16:24].unsqueeze(2).to_broadcast([128, 8, 32]), op=ALU.mult)
                yield
                E("dve", "tensor_tensor", [R("mb0"), R("pen")], [R("mb")], out=mb3, in0=mb3,
                  in1=sm_[:, 24:32].unsqueeze(2).to_broadcast([128, 8, 32]), op=ALU.add)
                yield
                E("dve", "max", [R("mb")], [R("t8")], out=sm_[:, 32:40], in_=mb_[:])
                yield
                E("dve", "tensor_scalar", [R("mb"), R("t8")], [f"selb{ti}"], out=selb, in0=mb_[:], scalar1=sm_[:, 39:40],
                  scalar2=None, op0=ALU.is_ge)
                yield
                E("dve", "scalar_tensor_tensor", [R("sc"), f"selb{ti}"], [R("g0t"), R("den")], out=g0_[:], in0=sc_[:],
                  scalar=1.0, in1=selb, op0=ALU.mult, op1=ALU.mult, accum_out=sm_[:, 40:41])
                yield
                E("dve", "reciprocal", [R("den")], [R("rden")], out=sm_[:, 41:42], in_=sm_[:, 40:41])
                MM(cum, tri[:], selb, True, True, [f"selb{ti}", "const"], ["cc"])
                MM(csum, onesb[:], selb, True, True, [f"selb{ti}", "const"], ["cc"])
                E("dve", "tensor_tensor", ["cc", "carryE"], [R("S1a")], out=S1_[:], in0=cum, in1=carryE[:], op=ALU.add)
                E("dve", "tensor_tensor", ["cc", R("S1a")], ["carryE"], out=carryE[:], in0=csum, in1=carryE[:], op=ALU.add)
                yield
                E("dve", "tensor_tensor", [R("S1a"), f"selb{ti}"], [R("S1")], out=S1_[:], in0=S1_[:], in1=selb, op=ALU.mult)
                yield
                E("dve", "max", [R("S1")], [R("sl8")], out=sm_[:, 48:56], in_=S1_[:])
                yield
                for k in range(8):
                    E("dve", "scalar_tensor_tensor", [R("S1"), R("sl8"), R("g0t")], [R("junk2"), R(f"gk{k}_")], out=j2_[:],
                      in0=S1_[:], scalar=sm_[:, 48 + k:49 + k], in1=g0_[:], op0=ALU.is_equal, op1=ALU.mult,
                      accum_out=sm_[:, 56 + k:57 + k])
                    if k % 2 == 1:
                        yield
                E("dve", "tensor_scalar", [R(f"gk{k}_") for k in range(8)] + [R("rden")], [f"gates{ti}"],
                  out=gates_all[:, ti, :], in0=sm_[:, 56:64], scalar1=sm_[:, 41:42], scalar2=2.5, op0=ALU.mult,
                  op1=ALU.mult)

            run_interleaved((bodyB(ti) for ti in range(NT)), 3)
            S.barrier_all()
        with ExitStack() as s2:
            eCt = sb(s2, "eCt", [128, NE], F32)
            pbf = sb(s2, "pbf", [128, NE], F32)
            pfx = [sb(s2, f"pfx{i}", [128, 128 + NE], F32) for i in range(2)]
            carry2 = sb(s2, "carry2", [128, NE], F32)
            junkc = sb(s2, "junkc", [128, NE], F32)
            becol = sb(s2, "becol", [128, 4], F32)
            dg = sb(s2, "dg", [128, 128], BF)
            S2 = sb(s2, "S2", [128, NE], F32)
            sl8 = sb(s2, "sl8", [128, 8], F32)
            h2r = [sb(s2, f"h2r{i}", [128, D], BF) for i in range(3)]
            pcc = ps(s2, "pcc", [128, 512])
            pbe = ps(s2, "pbe", [128, 512])
            DMA("sp", eCt[:], eC_d, [], ["eCt"], "ld_c")
            E("pool", "memset", [], ["pfx0"], pfx[0][:], 0.0)
            E("pool", "memset", [], ["pfx1"], pfx[1][:], 0.0)
            E("dve", "tensor_tensor", ["eCt"], ["cntf"], out=junkc[:], in0=carryE[:], in1=eCt[:], op=ALU.subtract)
            E("pool", "memset", [], ["pbf"], pbf[:], 0.0)
            for m_ in range(16):
                E("dve", "scalar_tensor_tensor", ["cntf", "pbf"], ["pbf"], out=pbf[:], in0=junkc[:], scalar=float(BLK * m_),
                  in1=pbf[:], op0=ALU.is_gt, op1=ALU.add)
            E("dve", "tensor_copy", ["pbf", "pfx0"], ["pfx0"], out=pfx[0][:, 128:], in_=pbf[:])
            cur = 0
            for sft in (1, 2, 4, 8, 16, 32, 64, 128):
                E("dve", "tensor_tensor", [f"pfx{cur}", f"pfx{1 - cur}"], [f"pfx{1 - cur}"], out=pfx[1 - cur][:, 128:],
                  in0=pfx[cur][:, 128:], in1=pfx[cur][:, 128 - sft:128 + NE - sft], op=ALU.add)
                cur = 1 - cur
            pend = pfx[cur][:, 128:]
            E("dve", "tensor_tensor", [f"pfx{cur}", "pbf"], ["c2a"], out=carry2[:], in0=pend, in1=pbf[:], op=ALU.subtract)
            E("dve", "tensor_scalar", ["c2a"], ["carry2"], out=carry2[:], in0=carry2[:], scalar1=float(BLK), scalar2=None,
              op0=ALU.mult)
            for jh in range(3):
                E("dve", "tensor_scalar", [f"pfx{cur}", "const"], ["junkd", f"be{jh}"], out=S2[:], in0=pend,
                  scalar1=jcol[:, jh:jh + 1], scalar2=0.0, op0=ALU.is_le, op1=ALU.add, accum_out=becol[:, jh:jh + 1])
                E("dve", "tensor_scalar", [f"be{jh}", "const"], ["dg"], out=dg[:], in0=ident[:], scalar1=becol[:, jh:jh + 1],
                  scalar2=None, op0=ALU.mult)
                MM(pbe[:, 0:128], onesb[:], dg[:], True, True, ["dg", "const"], ["pbe"])
                E("dve", "tensor_scalar", ["pbe", "const"], [], out=widx[:, jh * 128:(jh + 1) * 128], in0=pbe[:, 0:128],
                  scalar1=128.0, scalar2=jcol[:, 0:1], op0=ALU.mult, op1=ALU.add, shared=["widx"])
            for ti in range(NT):
                tsl = slice(ti * 128, (ti + 1) * 128)
                selb = selb_all[:, ti, :]
                hr = h2r[ti % 3]
                DMA("sp", hr[:], h2d[tsl, :], [], [f"h2r{ti % 3}"], f"ld_x{ti % 3}")
                cum = pcc[:, 0:256]
                csum = pcc[:, 256:512]
                MM(cum, tri[:], selb, True, True, ["const"], ["pcc"])
                MM(csum, onesb[:], selb, True, True, ["const"], ["pcc"])
                E("dve", "tensor_tensor", ["pcc", "carry2"], ["S2a"], out=S2[:], in0=cum, in1=carry2[:], op=ALU.add)
                E("dve", "tensor_tensor", ["S2a"], ["S2"], out=S2[:], in0=S2[:], in1=selb, op=ALU.mult)
                E("dve", "tensor_tensor", ["pcc", "S2a"], ["carry2"], out=carry2[:], in0=csum, in1=carry2[:], op=ALU.add)
                E("dve", "max", ["S2"], ["sl8"], out=sl8[:], in_=S2[:])
                E("dve", "tensor_scalar", ["sl8"], [f"slots{ti}"], out=slots_all[:, ti, :], in0=sl8[:], scalar1=-1.0,
                  scalar2=None, op0=ALU.add)
                for k in range(8):
                    S.op("pool", (lambda e, ti=ti, k=k, hr=hr: e.indirect_dma_start(
                        out=xg, out_offset=bass.IndirectOffsetOnAxis(ap=slots_all[:, ti, k:k + 1], axis=0),
                        in_=hr[:, :], in_offset=None)),
                        reads=[f"slots{ti}", f"h2r{ti % 3}"], shared=["xg"], dma=f"scat{ti % 3}_{k}")
            if "route" in dbg:
                d_ = dbgt("slots", [128, NT * 8], I32)
                DMA("sp", d_, slots_all[:].rearrange("p a b -> p (a b)"), [f"slots{t}" for t in range(NT)], [], "dbg")
                d_ = dbgt("gates", [128, NT * 8], F32)
                DMA("sp", d_, gates_all[:].rearrange("p a b -> p (a b)"), [], [], "dbg")
                d_ = dbgt("widx", [128, NBLK], I32)
                DMA("sp", d_, widx[:], ["widx"], [], "dbg")
            S.barrier_all()
        if stop_after == 2:
            S.emit(main)
            return nc, dbg_out

        with ExitStack() as sC:
            NB = 4
            wg2 = [sb(sC, f"wg{i}", [128, 2048], BF) for i in range(NB)]
            wu2 = [sb(sC, f"wu{i}", [128, 2048], BF) for i in range(NB)]
            wd2 = [sb(sC, f"wd{i}", [128, 2048], BF) for i in range(NB)]
            wg = [t[:].rearrange("p (k f) -> p k f", k=8) for t in wg2]
            wu = [t[:].rearrange("p (k f) -> p k f", k=8) for t in wu2]
            wd = [t[:].rearrange("p (j d) -> p j d", j=2) for t in wd2]
            Xe = [sb(sC, f"Xe{i}", [128, 2, D], BF) for i in range(NB)]
            XT = [sb(sC, f"XT{i}", [128, 8, 256], BF) for i in range(2)]
            sgL = [sb(sC, f"sgC{i}", [128, 2, 256], F32) for i in range(2)]
            HT = [sb(sC, f"HT{i}", [128, 2, 256], BF) for i in range(2)]
            Ys = [sb(sC, f"Ys{i}", [128, 2, D], BF) for i in range(2)]
            pxtL = [ps(sC, f"pxt{i}", [128, 2048], BF) for i in range(2)]
            phgu = ps(sC, "phgu", [128, 1024])
            pyL = [ps(sC, f"py{i}", [128, 512]) for i in range(2)]
            bc_reg = {}
            wgv = w_gate.rearrange("e (p k) f -> (e p) (k f)", p=128)
            wuv = w_up.rearrange("e (p k) f -> (e p) (k f)", p=128)
            wdv = w_down.rearrange("e (p j) d -> (e p) (j d)", p=128)
            NBL = n_experts if n_experts != NE else NBLK
            hg = phgu[:, 0:512].rearrange("p (a b) -> p a b", a=2)
            hu = phgu[:, 512:1024].rearrange("p (a b) -> p a b", a=2)

            order = []
            lo_, hi_ = 0, NBL - 1
            while lo_ <= hi_:
                order.append(lo_)
                if hi_ != lo_:
                    order.append(hi_)
                lo_ += 1
                hi_ -= 1

            def c_load(p_):
                j_ = order[p_]
                b3 = p_ % NB
                for (dst, src, nm) in ((wg2[b3], wgv, "wg"), (wu2[b3], wuv, "wu"), (wd2[b3], wdv, "wd")):
                    def wgather(e, dst=dst, src=src, j_=j_):
                        if "r" not in bc_reg:
                            bc_reg["r"] = e.to_reg(NE * 128 - 1)
                        return e.indirect_dma_start(
                            out=dst[:, :], out_offset=None, in_=src,
                            in_offset=bass.IndirectOffsetOnAxis(ap=widx[:, j_:j_ + 1], axis=0),
                            bounds_check=bc_reg["r"], oob_is_err=False)
                    S.op("pool", wgather,
                        reads=["widx"], writes=[f"{nm}{b3}"], dma=f"ld_{nm}{b3}")
                DMA("sp", Xe[b3][:], xg[j_ * BLK:(j_ + 1) * BLK, :].rearrange("(s p) n -> p s n", p=128), ["xg"],
                    [f"Xe{b3}"], f"ld_xe{b3}")

            PF = 2

            def bodyC(p_):
                j_ = order[p_]
                b3 = p_ % NB
                b2 = p_ % 2
                if p_ + PF < NBL:
                    c_load(p_ + PF)
                pxt = pxtL[b2]
                pxt3 = pxt[:].rearrange("p (k s) -> p k s", k=8)
                for s_ in range(2):
                    for k in range(8):
                        TR(pxt3[:, k, s_ * 128:(s_ + 1) * 128], Xe[b3][:, s_, k:k + 1017:8], [f"Xe{b3}"], [f"pxt{b2}"])
                E("dve", "tensor_copy", [f"pxt{b2}"], [f"XT{b2}"], out=XT[b2][:].rearrange("p a b -> p (a b)"), in_=pxt[:])
                yield
                for fc in range(2):
                    for k in range(8):
                        MM(hg[:, fc, :], wg[b3][:, k, fc:fc + 255:2], XT[b2][:, k, :], k == 0, k == 7,
                           [f"wg{b3}", f"XT{b2}"], ["hgC"])
                for fc in range(2):
                    for k in range(8):
                        MM(hu[:, fc, :], wu[b3][:, k, fc:fc + 255:2], XT[b2][:, k, :], k == 0, k == 7,
                           [f"wu{b3}", f"XT{b2}"], ["huC"])
                E("act", "activation", ["hgC"], [f"sgC{b2}"], out=sgL[b2][:], in_=hg, func=AF.Silu)
                E("dve", "tensor_tensor", [f"sgC{b2}", "huC"], [f"HT{b2}"], out=HT[b2][:], in0=sgL[b2][:], in1=hu, op=ALU.mult)
                yield
                for s_ in range(2):
                    for hf in range(2):
                        pi = hf
                        for fc in range(2):
                            MM(pyL[pi][:, :], HT[b2][:, fc, s_ * 128:(s_ + 1) * 128],
                               wd[b3][:, fc, hf * 512:(hf + 1) * 512], fc == 0, fc == 1, [f"HT{b2}", f"wd{b3}"],
                               [f"py{pi}"])
                        E("dve", "tensor_tensor", [f"py{pi}"], [], out=Ys[b2][:, s_, hf * 512:(hf + 1) * 512], in0=pyL[pi][:, :],
                          in1=gate2_b[:, hf * 512:(hf + 1) * 512], op=ALU.mult, shared=[f"Ys{b2}"])
                    if s_ == 0:
                        yield
                DMA("sp", ysd[j_ * BLK:(j_ + 1) * BLK, :].rearrange("(s p) n -> p s n", p=128), Ys[b2][:], [f"Ys{b2}"], [],
                    f"st_ys{b2}", shared=["ysd"])

            for j_ in range(min(PF, NBL)):
                c_load(j_)
            run_interleaved((bodyC(j_) for j_ in range(NBL)), 2)
            S.barrier_all()
        if stop_after == 3:
            S.emit(main)
            return nc, dbg_out

        with ExitStack() as sD:
            G = [[sb(sD, f"G{i}_{k}", [128, D], BF) for k in range(8)] for i in range(3)]
            bt = [sb(sD, f"btD{i}", [128, D], F32) for i in range(3)]
            accD = [sb(sD, f"accD{i}", [128, D], F32) for i in range(3)]
            ot = [sb(sD, f"ot{i}", [128, D], F32) for i in range(3)]
            gfin = sb(sD, "gfin", [128, D], F32)
            junk = [sb(sD, f"junkD{i}", [128, D], BF) for i in range(3)]
            mhalfD = sb(sD, "mhalfD", [128, 1], F32)
            DMA("sp", gfin[:], gfin_b, [], ["gfin"], "ld_c")
            E("pool", "memset", [], ["mhalfD"], mhalfD[:], -0.5)

            def bodyD(ti):
                tsl = slice(ti * 128, (ti + 1) * 128)
                b2 = ti % 3
                a_ = accD[b2]
                DMA("sp", bt[b2][:], out[tsl, :], ["outd"], [f"btD{b2}"], f"ld_x{b2}")
                for k in range(8):
                    S.op("pool", (lambda e, ti=ti, k=k, b2=b2: e.indirect_dma_start(
                        out=G[b2][k][:, :], out_offset=None, in_=ysd,
                        in_offset=bass.IndirectOffsetOnAxis(ap=slots_all[:, ti, k:k + 1], axis=0))),
                        reads=["ysd"], writes=[f"G{b2}_{k}"], dma=f"scat{b2}_{k}")
                yield
                E("dve", "scalar_tensor_tensor", [f"G{b2}_0", f"btD{b2}"], [f"accD{b2}"], out=a_[:], in0=G[b2][0][:],
                  scalar=gates_all[:, ti, 0:1], in1=bt[b2][:], op0=ALU.mult, op1=ALU.add)
                yield
                for k in range(1, 8):
                    E("dve", "scalar_tensor_tensor", [f"G{b2}_{k}"], [f"accD{b2}"], out=a_[:], in0=G[b2][k][:],
                      scalar=gates_all[:, ti, k:k + 1], in1=a_[:], op0=ALU.mult, op1=ALU.add)
                    yield
                E("act", "activation", [f"accD{b2}"], [f"junkD{b2}", f"ssD{b2}"], out=junk[b2][:], in_=a_[:], func=AF.Square,
                  accum_out=stat[:, 0, ti:ti + 1])
                yield
                E("dve", "tensor_scalar", [f"ssD{b2}"], [f"msD{b2}"], out=stat[:, 1, ti:ti + 1], in0=stat[:, 0, ti:ti + 1],
                  scalar1=1.0 / D, scalar2=EPS, op0=ALU.mult, op1=ALU.add)
                yield
                E("act", "activation", [f"msD{b2}"], [f"sqD{b2}"], out=stat[:, 2, ti:ti + 1], in_=stat[:, 1, ti:ti + 1],
                  func=AF.Sqrt)
                yield
                E("dve", "reciprocal", [f"sqD{b2}"], [f"rstdD{b2}"], out=stat[:, 3, ti:ti + 1], in_=stat[:, 2, ti:ti + 1])
                yield
                E("dve", "scalar_tensor_tensor", [f"accD{b2}", f"rstdD{b2}", "gfin"], [f"ot{b2}"], out=ot[b2][:], in0=a_[:],
                  scalar=stat[:, 3, ti:ti + 1], in1=gfin[:], op0=ALU.mult, op1=ALU.mult)
                DMA("sp", out[tsl, :], ot[b2][:], [f"ot{b2}", f"btD{b2}"], [], f"st_b{b2}", shared=["outf"])

            run_interleaved((bodyD(ti) for ti in range(NT)), 3)
            S.barrier_all()
        S.emit(main)
    return nc, dbg_out


def host_inputs(inputs, b):
    f = np.float32
    bf = ml_dtypes.bfloat16
    c = np.asarray(inputs["c"][b], f)
    b_ada = np.asarray(inputs["b_ada"][0], f)
    pool_w = np.asarray(inputs["pool_w"][0], f)
    pw_bd = np.zeros((128, 2, 128), f)
    for g in range(4):
        ch, gl = divmod(g, 2)
        pw_bd[gl * 64:(gl + 1) * 64, ch, gl * 64:(gl + 1) * 64] = pool_w[g]
    kk = np.arange(128)
    mask = np.zeros((128, 2, 128), f)
    mask[:, 0, :] = np.where(kk[:, None] >= kk[None, :], 1.0, 0.0)
    mask[:, 1, :] = np.where(kk[:, None] <= kk[None, :], 1.0, 0.0)
    wins = np.array([2, 4, 8, 16], f)
    invw = np.zeros((128, 2), f)
    invcnt = np.zeros((128, 2, 16), f)
    for g in range(4):
        ch, gl = divmod(g, 2)
        invw[gl * 64:(gl + 1) * 64, ch] = 1.0 / wins[g]
        invcnt[gl * 64:(gl + 1) * 64, ch, :] = 1.0 / np.minimum(np.arange(16) + 1, wins[g])
    d = {
        "x": np.ascontiguousarray(inputs["x"][b], f),
        "c_col": np.ascontiguousarray(c.reshape(8, 128).T),
        "w_ada": np.asarray(inputs["w_ada"][0], f),
        "b_col": np.ascontiguousarray(b_ada[:2048].reshape(16, 128).T),
        "b_row": np.ascontiguousarray(b_ada.reshape(1, -1)),
        "gmix_col": np.ascontiguousarray(np.asarray(inputs["g_mix"][0], f).reshape(8, 128).T),
        "gffn_b": np.ascontiguousarray(np.broadcast_to(np.asarray(inputs["g_ffn"][0], f), (128, D))),
        "gfin_b": np.ascontiguousarray(np.broadcast_to(np.asarray(inputs["g_final"], f), (128, D))),
        "w_in": np.asarray(inputs["w_in"][0], f),
        "pw_bd": pw_bd,
        "pscale_col": np.ascontiguousarray(np.asarray(inputs["pool_scale"][0], f).reshape(2, 128).T),
        "w_out": np.asarray(inputs["w_out"][0], f),
        "w_router": np.asarray(inputs["w_router"][0], f),
        "rbias_b": np.ascontiguousarray(np.broadcast_to(np.asarray(inputs["router_bias"][0], f), (128, NE))),
        "w_gate": np.asarray(inputs["w_gate"][0], f),
        "w_up": np.asarray(inputs["w_up"][0], f),
        "w_down": np.asarray(inputs["w_down"][0], f),
        "ws_gate": np.asarray(inputs["ws_gate"][0], f),
        "ws_up": np.asarray(inputs["ws_up"][0], f),
        "ws_down": np.asarray(inputs["ws_down"][0], f),
        "ident_bf": np.eye(128, dtype=f).astype(bf),
        "tri_bf": (kk[:, None] <= kk[None, :]).astype(f).astype(bf),
        "ones_bf": np.ones((128, 128), f).astype(bf),
        "mask_bf": mask.astype(bf),
        "ones_f": np.ones((128, 128), f),
        "eC_b": np.ascontiguousarray(np.broadcast_to((np.arange(NE) * EBIG).astype(f), (128, NE))),
        "jcol": np.ascontiguousarray((np.arange(3)[None, :] * 128 + np.arange(128)[:, None]).astype(f)),
        "invw_col": invw,
        "invcnt": invcnt,
    }
    return d


_NC_CACHE = {}


def kernel(**inputs):
    n = 8
    if "nc" not in _NC_CACHE:
        _NC_CACHE["nc"] = build_nc()[0]
    nc = _NC_CACHE["nc"]
    in_maps = [host_inputs(inputs, b) for b in range(n)]
    res = run_bass_kernel_spmd(nc, in_maps, core_ids=list(range(n)))
    return np.stack([np.asarray(r["out"], np.float32) for r in res.results], axis=0)
```
